# Optimizing a Trainium2 kernel written in Bass

```python
import math
import jax, jax.numpy as jnp
from jax import lax
import numpy as np

D_MODEL = 2048
BATCH = 2
SEQ = 8192
DEPTH = 2

N_MIXERS = 2
N_DIFF_LAYERS = (DEPTH + 1) // 2
N_NSA_LAYERS = DEPTH // 2

HEAD_DIM = 128
ROPE_THETA = 10000.0
Q_BLOCK = 128

DIFF_HEADS = D_MODEL // (2 * HEAD_DIM)
DIFF_V_DIM = 2 * HEAD_DIM
DIFF_IN = 4 * DIFF_HEADS * HEAD_DIM + DIFF_HEADS * DIFF_V_DIM

NSA_HEADS = D_MODEL // HEAD_DIM
NSA_GROUPS = 4
NSA_HPG = NSA_HEADS // NSA_GROUPS
NSA_KV = NSA_GROUPS * HEAD_DIM
N_BRANCH = 3
CMP_LEN = 32
CMP_STRIDE = 16
SEL_LEN = 64
N_SELECT = 16
WINDOW = 512
NSA_IN = NSA_HEADS * HEAD_DIM + N_BRANCH * 2 * NSA_KV + NSA_HEADS * N_BRANCH

N_EXPERTS = 32
TOP_K = 4
D_FF = D_MODEL
SWIGLU_LIMIT = 7.0
SWIGLU_ALPHA = 1.702
MOE_BLOCK = 128

DEEPNORM_ALPHA = (2 * DEPTH) ** 0.25
DEEPNORM_BETA = (8 * DEPTH) ** -0.25
LN_EPS = 1e-5
RMS_EPS = 1e-5
NEG_INF = -1e30

kernel_name = 'hybrid_diffattn_nsa_moe_deepnorm'


def layer_norm(x, g, b):
    xf = x.astype(jnp.float32)
    mu = jnp.mean(xf, -1, keepdims=True)
    xc = xf - mu
    var = jnp.mean(xc * xc, -1, keepdims=True)
    return (xc * lax.rsqrt(var + LN_EPS) * g + b).astype(x.dtype)


def rope_tables(positions):
    inv = ROPE_THETA ** (-jnp.arange(0, HEAD_DIM, 2, dtype=jnp.float32) / HEAD_DIM)
    ang = positions.astype(jnp.float32)[..., None] * inv
    return jnp.cos(ang)[:, :, None, :], jnp.sin(ang)[:, :, None, :]


def apply_rope(t, cos, sin):
    half = t.shape[-1] // 2
    t1 = t[..., :half].astype(jnp.float32)
    t2 = t[..., half:].astype(jnp.float32)
    return jnp.concatenate([t1 * cos - t2 * sin, t2 * cos + t1 * sin], -1).astype(t.dtype)


def masked_softmax(s, mask):
    p = jax.nn.softmax(jnp.where(mask, s, NEG_INF), axis=-1)
    return jnp.where(mask, p, 0.0)


def diff_attention(x, cos, sin, w_in, lq1, lk1, lq2, lk2, subln_g, w_o, lambda_init):
    B, S, _ = x.shape
    H, dh = DIFF_HEADS, HEAD_DIM
    nq = S // Q_BLOCK
    proj = x @ w_in
    w = H * dh
    q1, q2, k1, k2 = [apply_rope(proj[..., i * w:(i + 1) * w].reshape(B, S, H, dh), cos, sin)
                      for i in range(4)]
    v = proj[..., 4 * w:].reshape(B, S, H, DIFF_V_DIM)
    q = jnp.stack([q1, q2], axis=2)
    k = jnp.stack([k1, k2], axis=2)
    lam = (jnp.exp(jnp.sum(lq1.astype(jnp.float32) * lk1.astype(jnp.float32)))
           - jnp.exp(jnp.sum(lq2.astype(jnp.float32) * lk2.astype(jnp.float32))) + lambda_init)
    scale = dh ** -0.5
    qb_all = q.reshape(B, nq, Q_BLOCK, 2, H, dh).transpose(1, 0, 2, 3, 4, 5)
    kpos = jnp.arange(S)

    def block(args):
        qb, i = args
        t = i * Q_BLOCK + jnp.arange(Q_BLOCK)
        s = jnp.einsum('bqmhd,bkmhd->bmhqk', qb, k).astype(jnp.float32) * scale
        mask = kpos[None, :] <= t[:, None]
        p = jax.nn.softmax(jnp.where(mask, s, NEG_INF), axis=-1)
        a = p[:, 0] - lam * p[:, 1]
        return jnp.einsum('bhqk,bkhv->bqhv', a.astype(v.dtype), v)

    out = lax.map(block, (qb_all, jnp.arange(nq)))
    out = out.transpose(1, 0, 2, 3, 4).reshape(B, S, H, DIFF_V_DIM).astype(jnp.float32)
    out = out * lax.rsqrt(jnp.mean(out * out, -1, keepdims=True) + RMS_EPS) * subln_g
    out = (out * (1.0 - lambda_init)).astype(x.dtype)
    return out.reshape(B, S, H * DIFF_V_DIM) @ w_o


def selection_scores(p_cmp, n_sel):
    R = SEL_LEN // CMP_STRIDE
    C = CMP_LEN // CMP_STRIDE
    n_cmp = p_cmp.shape[-1]
    right = R * n_sel + R + C - n_cmp
    pp = jnp.pad(p_cmp, [(0, 0)] * (p_cmp.ndim - 1) + [(C - 1, right)])
    out = jnp.zeros(p_cmp.shape[:-1] + (n_sel,), p_cmp.dtype)
    for m in range(R):
        for n in range(C):
            start = C - 1 + m - n
            out = out + pp[..., start:start + R * n_sel:R]
    return out


def nsa_attention(x, cos, sin, w_in, ck_pos, ck_w1, ck_w2, cv_pos, cv_w1, cv_w2, w_o):
    B, S, _ = x.shape
    H, G, hpg, dh = NSA_HEADS, NSA_GROUPS, NSA_HPG, HEAD_DIM
    nq = S // Q_BLOCK
    n_cmp = (S - CMP_LEN) // CMP_STRIDE + 1
    n_sel = S // SEL_LEN
    n_top = min(N_SELECT, n_sel)
    scale = dh ** -0.5
    proj = x @ w_in
    off = H * dh
    q = apply_rope(proj[..., :off].reshape(B, S, H, dh), cos, sin)
    kv = proj[..., off:off + 6 * NSA_KV].reshape(B, S, N_BRANCH, 2, G, dh)
    off = off + 6 * NSA_KV
    gates = jax.nn.sigmoid(proj[..., off:].astype(jnp.float32)).reshape(B, S, H, N_BRANCH)
    k_cmp, k_sel, k_win = [apply_rope(kv[:, :, c, 0], cos, sin) for c in range(N_BRANCH)]
    v_cmp, v_sel, v_win = [kv[:, :, c, 1] for c in range(N_BRANCH)]

    cidx = jnp.arange(n_cmp)[:, None] * CMP_STRIDE + jnp.arange(CMP_LEN)[None, :]

    def compress(t, pos, w1, w2):
        blocks = t[:, cidx] + pos[None, None, :, None, :]
        flat = blocks.transpose(0, 1, 3, 2, 4).reshape(B, n_cmp, G, CMP_LEN * dh)
        return (jax.nn.silu(flat @ w1) @ w2).transpose(0, 2, 1, 3)

    kc = compress(k_cmp, ck_pos, ck_w1, ck_w2)
    vc = compress(v_cmp, cv_pos, cv_w1, cv_w2)
    ks_b = k_sel.transpose(0, 2, 1, 3).reshape(B, G, n_sel, SEL_LEN, dh)
    vs_b = v_sel.transpose(0, 2, 1, 3).reshape(B, G, n_sel, SEL_LEN, dh)
    kw_pad = jnp.pad(k_win.transpose(0, 2, 1, 3), ((0, 0), (0, 0), (WINDOW, 0), (0, 0)))
    vw_pad = jnp.pad(v_win.transpose(0, 2, 1, 3), ((0, 0), (0, 0), (WINDOW, 0), (0, 0)))
    qg = q.reshape(B, S, G, hpg, dh).transpose(0, 2, 3, 1, 4)
    cend = jnp.arange(n_cmp) * CMP_STRIDE + CMP_LEN - 1
    blk = jnp.arange(n_sel)
    bi = jnp.arange(B)[:, None, None, None]
    gi = jnp.arange(G)[None, :, None, None]

    def block(i):
        s0 = i * Q_BLOCK
        t = s0 + jnp.arange(Q_BLOCK)
        qb = lax.dynamic_slice_in_dim(qg, s0, Q_BLOCK, axis=3)
        sc = jnp.einsum('bghqd,bgnd->bghqn', qb, kc).astype(jnp.float32) * scale
        pc = masked_softmax(sc, cend[None, :] <= t[:, None])
        o_cmp = jnp.einsum('bghqn,bgnd->bghqd', pc.astype(vc.dtype), vc)
        imp = selection_scores(jnp.sum(pc, axis=2), n_sel)
        cur = (t // SEL_LEN)[:, None]
        valid = blk[None, :] * SEL_LEN <= t[:, None]
        forced = (blk[None, :] == 0) | (blk[None, :] == cur) | (blk[None, :] == cur - 1)
        score = jnp.where(valid, jnp.where(forced, jnp.inf, imp), -1.0)
        _, sel = lax.top_k(score, n_top)
        ks = ks_b[bi, gi, sel]
        vs = vs_b[bi, gi, sel]
        spos = sel[..., None] * SEL_LEN + jnp.arange(SEL_LEN)
        smask = (spos <= t[None, None, :, None, None]).reshape(B, G, Q_BLOCK, n_top * SEL_LEN)[:, :, None]
        ss = jnp.einsum('bghqd,bgqnld->bghqnl', qb, ks).astype(jnp.float32) * scale
        ps = masked_softmax(ss.reshape(B, G, hpg, Q_BLOCK, n_top * SEL_LEN), smask)
        o_sel = jnp.einsum('bghqnl,bgqnld->bghqd',
                           ps.reshape(B, G, hpg, Q_BLOCK, n_top, SEL_LEN).astype(vs.dtype), vs)
        kwb = lax.dynamic_slice_in_dim(kw_pad, s0, Q_BLOCK + WINDOW, axis=2)
        vwb = lax.dynamic_slice_in_dim(vw_pad, s0, Q_BLOCK + WINDOW, axis=2)
        wpos = s0 - WINDOW + jnp.arange(Q_BLOCK + WINDOW)
        wmask = ((wpos[None, :] <= t[:, None]) & (wpos[None, :] > t[:, None] - WINDOW)
                 & (wpos[None, :] >= 0))
        sw = jnp.einsum('bghqd,bgkd->bghqk', qb, kwb).astype(jnp.float32) * scale
        pw = masked_softmax(sw, wmask)
        o_win = jnp.einsum('bghqk,bgkd->bghqd', pw.astype(vwb.dtype), vwb)
        return jnp.stack([o_cmp, o_sel, o_win], axis=-2)

    outs = lax.map(block, jnp.arange(nq))
    outs = outs.transpose(1, 0, 4, 2, 3, 5, 6).reshape(B, S, H, N_BRANCH, dh)
    y = jnp.sum(outs.astype(jnp.float32) * gates[..., None], axis=3).astype(x.dtype)
    return y.reshape(B, S, H * dh) @ w_o


def moe_ffn(x, li, router_w, router_b, w_gu, b_gu, w_down, b_down):
    B, S, D = x.shape
    T = B * S
    xf = x.reshape(T, D)
    logits = (xf @ router_w[li] + router_b[li]).astype(jnp.float32)
    top_val, top_idx = lax.top_k(logits, TOP_K)
    gate = jax.nn.softmax(top_val, axis=-1)
    n_rows = T * TOP_K
    n_blocks = -(-(n_rows + N_EXPERTS * (MOE_BLOCK - 1)) // MOE_BLOCK)
    n_pad = n_blocks * MOE_BLOCK
    flat_e = top_idx.reshape(-1).astype(jnp.int32)
    flat_t = jnp.repeat(jnp.arange(T, dtype=jnp.int32), TOP_K)
    flat_w = gate.reshape(-1)
    counts = jnp.zeros((N_EXPERTS,), jnp.int32).at[flat_e].add(1)
    padded = ((counts + MOE_BLOCK - 1) // MOE_BLOCK) * MOE_BLOCK
    pad_end = jnp.cumsum(padded)
    pad_start = pad_end - padded
    grp_start = jnp.cumsum(counts) - counts
    order = jnp.argsort(flat_e, stable=True)
    se = flat_e[order]
    dest = pad_start[se] + (jnp.arange(n_rows, dtype=jnp.int32) - grp_start[se])
    row_tok = jnp.full((n_pad,), T, jnp.int32).at[dest].set(flat_t[order])
    row_w = jnp.zeros((n_pad,), jnp.float32).at[dest].set(flat_w[order])
    block_e = jnp.minimum(jnp.searchsorted(pad_end, jnp.arange(n_blocks) * MOE_BLOCK, side='right'),
                          N_EXPERTS - 1).astype(jnp.int32)
    x_pad = jnp.concatenate([xf, jnp.zeros((1, D), xf.dtype)], axis=0)
    x_rows = x_pad[row_tok].reshape(n_blocks, MOE_BLOCK, D)

    def expert_block(args):
        xb, e = args
        hgu = xb @ w_gu[li, e] + b_gu[li, e]
        g = jnp.minimum(hgu[:, :D_FF], SWIGLU_LIMIT)
        u = jnp.clip(hgu[:, D_FF:], -SWIGLU_LIMIT, SWIGLU_LIMIT)
        act = g * jax.nn.sigmoid(SWIGLU_ALPHA * g) * (u + 1.0)
        return act @ w_down[li, e] + b_down[li, e]

    y_rows = lax.map(expert_block, (x_rows, block_e)).reshape(n_pad, D)
    y = jnp.zeros((T + 1, D), x.dtype).at[row_tok].add(y_rows * row_w[:, None].astype(x.dtype))
    return y[:T].reshape(B, S, D)


def setup_inputs(seed: int = 0) -> dict:
    key = jax.random.key(seed)
    ks = jax.random.split(key, 24)
    f32 = jnp.float32
    D = D_MODEL

    def nrm(k, shape, scale):
        return jax.random.normal(k, shape, f32) * scale

    x = nrm(ks[0], (BATCH, SEQ, D), 1.0)
    positions = (jax.random.randint(ks[1], (BATCH, 1), 0, 1024, dtype=jnp.int32)
                 + jnp.arange(SEQ, dtype=jnp.int32)[None, :])
    ln_gain = 1.0 + nrm(ks[2], (DEPTH, 2, D), 0.02)
    ln_bias = nrm(ks[3], (DEPTH, 2, D), 0.02)
    diff_w_in = nrm(ks[4], (N_DIFF_LAYERS, D, DIFF_IN), D ** -0.5)
    lam_keys = jax.random.split(ks[5], 4)
    diff_lambda_q1 = nrm(lam_keys[0], (N_DIFF_LAYERS, HEAD_DIM), 0.1)
    diff_lambda_k1 = nrm(lam_keys[1], (N_DIFF_LAYERS, HEAD_DIM), 0.1)
    diff_lambda_q2 = nrm(lam_keys[2], (N_DIFF_LAYERS, HEAD_DIM), 0.1)
    diff_lambda_k2 = nrm(lam_keys[3], (N_DIFF_LAYERS, HEAD_DIM), 0.1)
    diff_subln_gain = 1.0 + nrm(ks[6], (N_DIFF_LAYERS, DIFF_V_DIM), 0.02)
    diff_w_o = nrm(ks[7], (N_DIFF_LAYERS, DIFF_HEADS * DIFF_V_DIM, D),
                   (DIFF_HEADS * DIFF_V_DIM) ** -0.5 * DEEPNORM_BETA)
    nsa_w_in = nrm(ks[8], (N_NSA_LAYERS, D, NSA_IN), D ** -0.5)
    nsa_cmp_k_pos = nrm(ks[9], (N_NSA_LAYERS, CMP_LEN, HEAD_DIM), 0.1)
    nsa_cmp_k_w1 = nrm(ks[10], (N_NSA_LAYERS, CMP_LEN * HEAD_DIM, HEAD_DIM), (CMP_LEN * HEAD_DIM) ** -0.5)
    nsa_cmp_k_w2 = nrm(ks[11], (N_NSA_LAYERS, HEAD_DIM, HEAD_DIM), HEAD_DIM ** -0.5)
    nsa_cmp_v_pos = nrm(ks[12], (N_NSA_LAYERS, CMP_LEN, HEAD_DIM), 0.1)
    nsa_cmp_v_w1 = nrm(ks[13], (N_NSA_LAYERS, CMP_LEN * HEAD_DIM, HEAD_DIM), (CMP_LEN * HEAD_DIM) ** -0.5)
    nsa_cmp_v_w2 = nrm(ks[14], (N_NSA_LAYERS, HEAD_DIM, HEAD_DIM), HEAD_DIM ** -0.5)
    nsa_w_o = nrm(ks[15], (N_NSA_LAYERS, NSA_HEADS * HEAD_DIM, D),
                  (NSA_HEADS * HEAD_DIM) ** -0.5 * DEEPNORM_BETA)
    moe_router_w = nrm(ks[16], (DEPTH, D, N_EXPERTS), D ** -0.5)
    moe_router_b = nrm(ks[17], (DEPTH, N_EXPERTS), 0.01)
    moe_w_gate_up = nrm(ks[18], (DEPTH, N_EXPERTS, D, 2 * D_FF), D ** -0.5)
    moe_b_gate_up = nrm(ks[19], (DEPTH, N_EXPERTS, 2 * D_FF), 0.01)
    moe_w_down = nrm(ks[20], (DEPTH, N_EXPERTS, D_FF, D), D_FF ** -0.5 * DEEPNORM_BETA)
    moe_b_down = nrm(ks[21], (DEPTH, N_EXPERTS, D), 0.01)
    return {'x': x, 'positions': positions, 'ln_gain': ln_gain, 'ln_bias': ln_bias,
            'diff_w_in': diff_w_in, 'diff_lambda_q1': diff_lambda_q1, 'diff_lambda_k1': diff_lambda_k1,
            'diff_lambda_q2': diff_lambda_q2, 'diff_lambda_k2': diff_lambda_k2,
            'diff_subln_gain': diff_subln_gain, 'diff_w_o': diff_w_o,
            'nsa_w_in': nsa_w_in, 'nsa_cmp_k_pos': nsa_cmp_k_pos, 'nsa_cmp_k_w1': nsa_cmp_k_w1,
            'nsa_cmp_k_w2': nsa_cmp_k_w2, 'nsa_cmp_v_pos': nsa_cmp_v_pos, 'nsa_cmp_v_w1': nsa_cmp_v_w1,
            'nsa_cmp_v_w2': nsa_cmp_v_w2, 'nsa_w_o': nsa_w_o,
            'moe_router_w': moe_router_w, 'moe_router_b': moe_router_b,
            'moe_w_gate_up': moe_w_gate_up, 'moe_b_gate_up': moe_b_gate_up,
            'moe_w_down': moe_w_down, 'moe_b_down': moe_b_down}


def reference(x, positions, ln_gain, ln_bias,
              diff_w_in, diff_lambda_q1, diff_lambda_k1, diff_lambda_q2, diff_lambda_k2,
              diff_subln_gain, diff_w_o,
              nsa_w_in, nsa_cmp_k_pos, nsa_cmp_k_w1, nsa_cmp_k_w2,
              nsa_cmp_v_pos, nsa_cmp_v_w1, nsa_cmp_v_w2, nsa_w_o,
              moe_router_w, moe_router_b, moe_w_gate_up, moe_b_gate_up, moe_w_down, moe_b_down):
    cos, sin = rope_tables(positions)
    h = x
    for li in range(DEPTH):
        j = li // N_MIXERS
        if li % N_MIXERS == 0:
            lambda_init = 0.8 - 0.6 * math.exp(-0.3 * li)
            mix = diff_attention(h, cos, sin, diff_w_in[j], diff_lambda_q1[j], diff_lambda_k1[j],
                                 diff_lambda_q2[j], diff_lambda_k2[j], diff_subln_gain[j],
                                 diff_w_o[j], lambda_init)
        else:
            mix = nsa_attention(h, cos, sin, nsa_w_in[j], nsa_cmp_k_pos[j], nsa_cmp_k_w1[j],
                                nsa_cmp_k_w2[j], nsa_cmp_v_pos[j], nsa_cmp_v_w1[j],
                                nsa_cmp_v_w2[j], nsa_w_o[j])
        h = layer_norm(DEEPNORM_ALPHA * h + mix, ln_gain[li, 0], ln_bias[li, 0])
        ffn = moe_ffn(h, li, moe_router_w, moe_router_b, moe_w_gate_up, moe_b_gate_up,
                      moe_w_down, moe_b_down)
        h = layer_norm(DEEPNORM_ALPHA * h + ffn, ln_gain[li, 1], ln_bias[li, 1])
    return h
```

```python
import numpy as np
import concourse.bass as bass
import concourse.mybir as mybir

F32 = mybir.dt.float32
BF16 = mybir.dt.bfloat16
I32 = mybir.dt.int32
U32 = mybir.dt.uint32
ALU = mybir.AluOpType
AF = mybir.ActivationFunctionType
AX = mybir.AxisListType

ENGS = ("pe", "act", "dve", "pool", "sp")


class Buf:
    __slots__ = ("name", "w", "r")

    def __init__(self, name):
        self.name = name
        self.w = None
        self.r = []


class Op:
    __slots__ = ("eng", "fn", "deps", "dma", "has_dep", "token", "idx")

    def __init__(self, eng, fn, deps, dma):
        self.eng = eng
        self.fn = fn
        self.deps = deps
        self.dma = dma
        self.has_dep = False
        self.token = None


class Sched:
    def __init__(self, nc, n_dma_sems=6, same_engine_sync=True):
        self.nc = nc
        self.ops = []
        self.n_dma_sems = n_dma_sems
        self.same_engine_sync = same_engine_sync
        self.nbuf = 0
        self.last = {}
        self.dma_open = []
        self.pending = {}

    def barrier(self):
        deps = list(self.last.values()) + list(self.dma_open)
        o = Op("sp", lambda e: e.nop(), deps, False)
        o.idx = len(self.ops)
        self.ops.append(o)
        self.dma_open = []
        self.last = {"sp": o}
        for e in ENGS:
            self.pending[e] = o
        return o

    def buf(self, name=None):
        self.nbuf += 1
        return Buf(name or f"b{self.nbuf}")

    def bufs(self, n, name="b"):
        return [self.buf(f"{name}{i}") for i in range(n)]

    def op(self, eng, fn, reads=(), writes=(), dma=False):
        deps = []
        seen = set()

        def add(o):
            if o is not None and id(o) not in seen:
                seen.add(id(o))
                deps.append(o)

        for b in reads:
            add(b.w)
        for b in writes:
            add(b.w)
            for r in b.r:
                add(r)
        if self.pending.get(eng) is not None:
            add(self.pending[eng])
            self.pending[eng] = None
        o = Op(eng, fn, deps, dma)
        o.idx = len(self.ops)
        if dma:
            self.dma_open.append(o)
        else:
            self.last[eng] = o
        for b in reads:
            b.r.append(o)
        for b in writes:
            b.w = o
            b.r = []
        self.ops.append(o)
        return o

    def pe(self, fn, reads=(), writes=()):
        return self.op("pe", fn, reads, writes)

    def act(self, fn, reads=(), writes=()):
        return self.op("act", fn, reads, writes)

    def dve(self, fn, reads=(), writes=()):
        return self.op("dve", fn, reads, writes)

    def pool(self, fn, reads=(), writes=()):
        return self.op("pool", fn, reads, writes)

    def dma(self, fn, reads=(), writes=(), q="sp"):
        return self.op(q, fn, reads, writes, dma=True)

    def finalize(self, final_wait_ops=()):
        nc = self.nc
        for o in self.ops:
            for d in o.deps:
                if d.eng == o.eng and not d.dma and (o.eng == "pe" or not self.same_engine_sync):
                    continue
                d.has_dep = True
        for o in final_wait_ops:
            o.has_dep = True
        used = sorted({o.eng for o in self.ops}, key=ENGS.index)
        sems = {e: nc.alloc_semaphore(f"s_{e}") for e in used}
        counts = {e: 0 for e in used}
        dma_sems = {}
        for e in used:
            if any(o.dma for o in self.ops if o.eng == e):
                dma_sems[e] = [[nc.alloc_semaphore(f"d_{e}{i}"), 0, None] for i in range(self.n_dma_sems)]
        dma_rr = {e: 0 for e in used}
        seen = {e: {} for e in used}
        prog = {e: [] for e in used}

        def need_wait(e, tok):
            sem, val = tok
            key = id(sem)
            if seen[e].get(key, 0) >= val:
                return
            seen[e][key] = val
            prog[e].append(("wait", sem, val))

        for o in self.ops:
            e = o.eng
            for d in o.deps:
                if d.eng == e and not d.dma and (e == "pe" or not self.same_engine_sync):
                    continue
                need_wait(e, d.token)
            if o.dma:
                slots = dma_sems[e]
                slot = slots[dma_rr[e] % len(slots)]
                dma_rr[e] += 1
                if slot[2] is not None:
                    need_wait(e, slot[2])
                slot[1] += 16
                o.token = (slot[0], slot[1])
                slot[2] = o.token
                prog[e].append(("op", o.fn, slot[0], 16))
            else:
                if o.has_dep:
                    counts[e] += 1
                    o.token = (sems[e], counts[e])
                    prog[e].append(("op", o.fn, sems[e], 1))
                else:
                    prog[e].append(("op", o.fn, None, 0))
        for o in final_wait_ops:
            need_wait("sp", o.token) if "sp" in prog else None
        handles = {"pe": nc.tensor, "act": nc.scalar, "dve": nc.vector, "pool": nc.gpsimd, "sp": nc.sync}

        def run(e):
            def body(eng):
                for it in prog[e]:
                    if it[0] == "wait":
                        eng.wait_ge(it[1], it[2])
                    else:
                        ins = it[1](eng)
                        if it[2] is not None:
                            ins.then_inc(it[2], it[3])
            return body

        with nc.Block() as block:
            for e in used:
                reg = {"pe": block.tensor, "act": block.scalar, "dve": block.vector,
                       "pool": block.gpsimd, "sp": block.sync}[e]
                reg(run(e))
        self.stats = {e: len(prog[e]) for e in used}
        self.maxcount = dict(counts)


class Arena:
    def __init__(self, nc, nbytes, name="arena"):
        self.nc = nc
        self.nbytes = nbytes
        self.t = nc.alloc_sbuf_tensor(name, [128, nbytes // 4], F32)
        self.off = 0
        self.peak = 0

    def mark(self):
        return self.off

    def reset(self, m):
        self.off = m

    def alloc(self, shape, dtype):
        esz = {F32: 4, I32: 4, U32: 4, BF16: 2}[dtype]
        n = 1
        for s in shape[1:]:
            n *= s
        nb = (n * esz + 31) // 32 * 32
        assert self.off + nb <= self.nbytes, f"arena overflow: {self.off + nb} > {self.nbytes}"
        ap = self.t[0:shape[0], self.off // 4:(self.off + nb) // 4]
        if dtype != F32:
            ap = ap.bitcast(dtype)
        ap = ap[:, 0:n]
        if len(shape) == 3:
            ap = ap.rearrange("p (a b) -> p a b", a=shape[1])
        elif len(shape) == 4:
            ap = ap.rearrange("p (a b c) -> p a b c", a=shape[1], b=shape[2])
        self.off += nb
        self.peak = max(self.peak, self.off)
        return ap


import numpy as np

BIGS = (1.0e4, 2.0e4, 4.0e4)

def perm(w):
    return np.concatenate([w[:, 64:], w[:, :64]], axis=1)

def rope_consts():
    p = np.arange(128)
    inv = 10000.0 ** (-(2.0 * (p % 64)) / 128.0)
    sgn = np.where(p < 64, -1.0, 1.0)
    return np.stack([inv / (2 * np.pi), 2 * np.pi * sgn], 1).astype(np.float32)

def causal_table():
    p = np.arange(128)[:, None]; c = np.arange(896)[None, :]
    return (p <= c - 384).astype(np.float32)

def nsa_consts(seq):
    ncmp = seq // 16 - 1
    p = np.arange(128)[:, None]
    c = np.arange(1408)[None, :]
    wm = ((c - 384 - p >= 0) & (c - 384 - p < 512)).astype(np.float32)
    u = np.arange(2560)[None, :]
    tc = (16 * p + 31 <= u).astype(np.float32)
    M = np.zeros((512, 128), np.float32)
    for j in range(128):
        for n, cf in ((4 * j - 1, 1), (4 * j, 2), (4 * j + 1, 2), (4 * j + 2, 2), (4 * j + 3, 1)):
            if 0 <= n < ncmp:
                M[n, j] = cf
    rc = np.zeros((128, 4, 129), np.float32)
    rc[:, :, :128] = M.reshape(4, 128, 128).transpose(1, 0, 2)
    nn = (np.arange(4)[None, :] * 128 + np.arange(128)[:, None])
    rc[:, :, 128] = (nn < ncmp)
    ones_l = np.ones((128, 128), np.float32); ones_l[127, :] = 0
    selg = np.ascontiguousarray(np.broadcast_to(np.eye(12, dtype=np.float32)[:, :, None], (12, 12, 128)))
    j = np.arange(128)[:, None, None]; kc = np.arange(64)[None, :, None]; k = np.arange(128)[None, None, :]
    ex = (j == 2 * kc + (k >= 64)).astype(np.float32)
    uu = np.arange(256)[None, :] - 127
    hi = (p >= 64).astype(np.int64)
    fadd = np.where(uu == hi, BIGS[0], 0.0) + np.where(uu == hi - 1, BIGS[1], 0.0)
    fval = (uu <= hi).astype(np.float32)
    return {"wm": wm, "tc": tc, "rc": rc, "ones_l": ones_l, "selg": selg, "ex": np.ascontiguousarray(ex),
            "fadd": fadd.astype(np.float32), "fval": fval, "ident": np.eye(128, dtype=np.float32),
            "cm": causal_table(), "invf": rope_consts()}

def nsa_weights(z, g):
    w_in = z['nsa_w_in'][0]
    wq = np.concatenate([np.concatenate([w_in[:, h * 128:(h + 1) * 128], perm(w_in[:, h * 128:(h + 1) * 128])], 1) for h in range(4 * g, 4 * g + 4)], 1)
    def kv(c, kvi):
        o = 2048 + ((c * 2 + kvi) * 4 + g) * 128
        return w_in[:, o:o + 128]
    wk = np.concatenate([np.concatenate([kv(c, 0), perm(kv(c, 0))], 1) for c in range(3)], 1)
    wv = np.concatenate([kv(c, 1) for c in range(3)], 1)
    wg = w_in[:, 5120 + 12 * g: 5120 + 12 * g + 12]
    w1 = np.stack([z['nsa_cmp_k_w1'][0], z['nsa_cmp_v_w1'][0]])
    w2 = np.stack([z['nsa_cmp_k_w2'][0], z['nsa_cmp_v_w2'][0]])
    cposT = np.stack([np.asarray(z['nsa_cmp_k_pos'][0]).T, np.asarray(z['nsa_cmp_v_pos'][0]).T])
    return {k: np.ascontiguousarray(v, dtype=np.float32) for k, v in
            {"wq": wq, "wk": wk, "wv": wv, "wg": wg, "w1": w1, "w2": w2, "cposT": cposT}.items()}

NSA_SHAPES = lambda seq: {"hT": [2048, seq], "wq": [2048, 1024], "wk": [2048, 768], "wv": [2048, 384], "wg": [2048, 12],
                          "w1": [2, 4096, 128], "w2": [2, 128, 128], "cposT": [2, 128, 32], "invf": [128, 2], "cm": [128, 896],
                          "wm": [128, 1408], "tc": [128, 2560], "rc": [128, 4, 129], "ones_l": [128, 128], "selg": [12, 12, 128],
                          "ex": [128, 64, 128], "fadd": [128, 256], "fval": [128, 256], "ident": [128, 128]}


D = 2048
NE = 32
TT = 512
ALPHA = 4 ** 0.25
LN_EPS = 1e-5
NW = 4


class WStream:
    def __init__(self, S, nc, n_buf, name="w"):
        self.S = S
        self.nc = nc
        self.n = n_buf
        self.tiles = [nc.alloc_sbuf_tensor(f"sb_{name}{i}", [128, 16, 512], BF16) for i in range(n_buf)]
        self.bufs = [S.buf(f"{name}{i}") for i in range(n_buf)]
        self.reqs = []
        self.issued = 0

    def request(self, src_ap):
        self.reqs.append(src_ap)
        return len(self.reqs) - 1

    def _issue(self, i):
        t = self.tiles[i % self.n]
        b = self.bufs[i % self.n]
        src = self.reqs[i].rearrange("(c p) n -> p c n", p=128)
        for h in range(2):
            self.S.dma(lambda e, t=t, src=src, h=h: e.dma_start(out=t[:, 8 * h:8 * h + 8, :], in_=src[:, 8 * h:8 * h + 8, :]),
                       writes=[b], q="pool")

    def get(self, i, hold=1):
        lim = min(len(self.reqs), i + self.n - hold + 1)
        while self.issued < lim:
            self._issue(self.issued)
            self.issued += 1
        return self.tiles[i % self.n], self.bufs[i % self.n]


def layer_norm_fm(S, nc, C, z, Bz, g_ap, b_ap, hb=None, Bhb=None):
    ones = C["ones_f"]
    psA, BpsA = C["ps_misc"][0]
    psB, BpsB = C["ps_misc"][1]
    nm, Bnm = C["ln_nm"]
    rstd, Brstd = C["ln_rstd"]

    def mm_sum(src_chunks, ps):
        def f(e):
            ins = None
            for c in range(16):
                ins = e.matmul(ps[:], ones[:], src_chunks(c), start=(c == 0), stop=(c == 15))
            return ins
        return f
    S.pe(mm_sum(lambda c: z[:, c, :], psA), reads=Bz + [C["Bconst"]], writes=[BpsA])
    S.act(lambda e: e.mul(nm[:], psA[:], -1.0 / D), reads=[BpsA], writes=[Bnm])
    sq = C["ln_sq"]
    for c in range(16):
        S.dve(lambda e, c=c: e.tensor_tensor(out=z[:, c, :], in0=z[:, c, :], in1=nm[:], op=ALU.add),
              reads=[Bz[c], Bnm], writes=[Bz[c]])
    for c in range(16):
        sqt, Bsq = sq[c % len(sq)]
        S.act(lambda e, c=c, sqt=sqt: e.activation(out=sqt[:], in_=z[:, c, :], func=AF.Square),
              reads=[Bz[c]], writes=[Bsq])
        S.pe(lambda e, c=c, sqt=sqt: e.matmul(psB[:], ones[:], sqt[:], start=(c == 0), stop=(c == 15)),
             reads=[Bsq, C["Bconst"]] + ([BpsB] if c else []), writes=[BpsB])
    S.dve(lambda e: e.tensor_scalar(out=rstd[:], in0=psB[:], scalar1=1.0 / D, scalar2=LN_EPS, op0=ALU.mult, op1=ALU.add),
          reads=[BpsB], writes=[Brstd])
    S.act(lambda e: e.sqrt(out=rstd[:], in_=rstd[:]), reads=[Brstd], writes=[Brstd])
    S.dve(lambda e: e.reciprocal(out=rstd[:], in_=rstd[:]), reads=[Brstd], writes=[Brstd])
    for c in range(16):
        S.dve(lambda e, c=c: e.scalar_tensor_tensor(out=z[:, c, :], in0=z[:, c, :], scalar=g_ap[:, c:c + 1], in1=rstd[:],
                                                    op0=ALU.mult, op1=ALU.mult),
              reads=[Bz[c], Brstd, C["Bconst"]], writes=[Bz[c]])
        if hb is not None:
            S.act(lambda e, c=c: e.activation(out=hb[:, c, :], in_=z[:, c, :], func=AF.Identity, bias=b_ap[:, c:c + 1], scale=1.0),
                  reads=[Bz[c], C["Bconst"]], writes=[Bhb[c]])
        S.dve(lambda e, c=c: e.tensor_scalar(out=z[:, c, :], in0=z[:, c, :], scalar1=b_ap[:, c:c + 1], scalar2=None, op0=ALU.add),
              reads=[Bz[c], C["Bconst"]], writes=[Bz[c]])


def emit_post(S, nc, IO, NT, li_tag=""):
    ntile = NT // TT
    A = lambda n, sh, dt: nc.alloc_sbuf_tensor("sb_" + n, sh, dt)
    z = A("z", [128, 16, TT], F32)
    hb = A("hb", [128, 16, TT], BF16)
    ab = A("ab", [128, 16, TT], BF16)
    lnp = A("lnp", [128, 4, 16], F32)
    rw = A("rw", [128, 16, 32], F32)
    rbb = A("rbb", [128, 32], F32)
    bgu = A("bgu", [128, 32, 32], F32)
    bd = A("bd", [128, 32, 16], F32)
    ident = A("ident", [128, 128], F32)
    sel = A("sel", [32, 32, 128], F32)
    ones_f = A("ones_f", [128, 128], F32)
    Bconst = S.buf("const")
    Bz = S.bufs(16, "z")
    Bhb = S.bufs(16, "hb")
    Bab = S.bufs(16, "ab")
    ps = [nc.alloc_psum_tensor(f"ps{i}", [128, 512], F32) for i in range(8)]
    Bps = S.bufs(8, "ps")
    C = {"ones_f": ones_f, "Bconst": Bconst,
         "ps_misc": [(ps[6], Bps[6]), (ps[7], Bps[7])],
         "ln_nm": (A("ln_nm", [128, TT], F32), S.buf()),
         "ln_rstd": (A("ln_rstd", [128, TT], F32), S.buf()),
         "ln_sq": [(A(f"ln_sq{i}", [128, TT], F32), S.buf()) for i in range(2)]}
    gs = [(A(f"gs{i}", [128, TT], F32), S.buf()) for i in range(2)]
    sg = [(A(f"sg{i}", [128, TT], F32), S.buf()) for i in range(2)]
    us = [(A(f"us{i}", [128, TT], F32), S.buf()) for i in range(2)]
    tmp = [(A(f"tmp{i}", [128, TT], F32), S.buf()) for i in range(2)]
    gwb = [(A(f"gwb{i}", [128, TT], F32), S.buf()) for i in range(2)]
    lg = A("lg", [128, 4, 32], F32)
    m8 = A("m8", [128, 4, 8], F32)
    ex = A("ex", [128, 4, 32], F32)
    mk = A("mk", [128, 4, 32], F32)
    rs = A("rs", [128, 4], F32)
    nmx = A("nmx", [128, 4], F32)
    gwT = A("gwT", [32, TT], F32)
    Blg, Bm8, Bex, Bmk, Brs, BgwT = S.bufs(6, "rt")

    cl = [
        S.dma(lambda e: e.dma_start(out=lnp[:], in_=IO["lnp"]), writes=[Bconst]),
        S.dma(lambda e: e.dma_start(out=rw[:], in_=IO["rw"].rearrange("(c p) n -> p c n", p=128)), writes=[Bconst]),
        S.dma(lambda e: e.dma_start(out=rbb[:], in_=IO["rbb"]), writes=[Bconst]),
        S.dma(lambda e: e.dma_start(out=bgu[:], in_=IO["bgu"]), writes=[Bconst]),
        S.dma(lambda e: e.dma_start(out=bd[:], in_=IO["bd"]), writes=[Bconst]),
        S.dma(lambda e: e.dma_start(out=ident[:], in_=IO["ident"]), writes=[Bconst]),
        S.dma(lambda e: e.dma_start(out=sel[:], in_=IO["sel"]), writes=[Bconst]),
    ]
    S.dve(lambda e: e.memset(ones_f[:], 1.0), writes=[Bconst])

    ws = WStream(S, nc, NW)
    req = {}
    for tt in range(ntile):
        for j in range(4):
            req[(tt, "wo", j)] = ws.request(IO["wo"][:, j * 512:(j + 1) * 512])
        for e_ in range(NE):
            for j in range(4):
                req[(tt, "g", e_, j)] = ws.request(IO["wgu"][e_, :, j * 512:(j + 1) * 512])
                req[(tt, "u", e_, j)] = ws.request(IO["wgu"][e_, :, D + j * 512:D + (j + 1) * 512])
            for j in range(4):
                req[(tt, "d", e_, j)] = ws.request(IO["wd"][e_, :, j * 512:(j + 1) * 512])

    outs = []
    pcnt = [0]

    def nextps(k=6):
        i = pcnt[0] % k
        pcnt[0] += 1
        return ps[i], Bps[i]

    for tt in range(ntile):
        tsl = slice(tt * TT, (tt + 1) * TT)
        xsrc = IO["xT"].rearrange("(c p) n -> p c n", p=128)
        asrc = IO["aT"].rearrange("(c p) n -> p c n", p=128)
        for h in range(2):
            S.dma(lambda e, h=h, tsl=tsl: e.dma_start(out=z[:, 8 * h:8 * h + 8, :], in_=xsrc[:, 8 * h:8 * h + 8, tsl]),
                  writes=Bz[8 * h:8 * h + 8])
            S.dma(lambda e, h=h, tsl=tsl: e.dma_start(out=ab[:, 8 * h:8 * h + 8, :], in_=asrc[:, 8 * h:8 * h + 8, tsl]),
                  writes=Bab[8 * h:8 * h + 8], q="pool")
        for j in range(4):
            wt, Bw = ws.get(req[(tt, "wo", j)])
            for dl in range(4):
                dc = j * 4 + dl
                p_, Bp = nextps()

                def mm(e, wt=wt, dl=dl, p_=p_):
                    ins = None
                    for kc in range(16):
                        ins = e.matmul(p_[:], wt[:, kc, dl * 128:(dl + 1) * 128], ab[:, kc, :], start=(kc == 0), stop=(kc == 15))
                    return ins
                S.pe(mm, reads=[Bw] + Bab, writes=[Bp])
                S.dve(lambda e, dc=dc, p_=p_: e.scalar_tensor_tensor(out=z[:, dc, :], in0=z[:, dc, :], scalar=ALPHA, in1=p_[:],
                                                                    op0=ALU.mult, op1=ALU.add),
                      reads=[Bz[dc], Bp], writes=[Bz[dc]])
        layer_norm_fm(S, nc, C, z, Bz, lnp[:, 0, :], lnp[:, 1, :], hb=hb, Bhb=Bhb)
        for tc in range(4):
            p_, Bp = C["ps_misc"][tc % 2]

            def mmr(e, tc=tc, p_=p_):
                ins = None
                for kc in range(16):
                    ins = e.matmul(p_[:, 0:32], z[:, kc, tc * 128:(tc + 1) * 128], rw[:, kc, :], start=(kc == 0), stop=(kc == 15))
                return ins
            S.pe(mmr, reads=Bz + [Bconst], writes=[Bp])
            S.dve(lambda e, tc=tc, p_=p_: e.tensor_tensor(out=lg[:, tc, :], in0=p_[:, 0:32], in1=rbb[:], op=ALU.add),
                  reads=[Bp, Bconst], writes=[Blg])
        for tc in range(4):
            S.dve(lambda e, tc=tc: e.max(out=m8[:, tc, :], in_=lg[:, tc, :]), reads=[Blg], writes=[Bm8])
        for tc in range(4):
            S.dve(lambda e, tc=tc: e.tensor_scalar(out=mk[:, tc, :], in0=lg[:, tc, :], scalar1=m8[:, tc, 3:4], scalar2=None, op0=ALU.is_ge),
                  reads=[Blg, Bm8], writes=[Bmk])
            S.dve(lambda e, tc=tc: e.tensor_scalar(out=nmx[:, tc:tc + 1], in0=m8[:, tc, 0:1], scalar1=-1.0, scalar2=None, op0=ALU.mult),
                  reads=[Bm8], writes=[Brs])
        for tc in range(4):
            S.act(lambda e, tc=tc: e.activation(out=ex[:, tc, :], in_=lg[:, tc, :], func=AF.Exp, bias=nmx[:, tc:tc + 1], scale=1.0),
                  reads=[Blg, Brs], writes=[Bex])
        for tc in range(4):
            S.dve(lambda e, tc=tc: e.tensor_tensor(out=ex[:, tc, :], in0=ex[:, tc, :], in1=mk[:, tc, :], op=ALU.mult),
                  reads=[Bex, Bmk], writes=[Bex])
            S.dve(lambda e, tc=tc: e.reduce_sum(out=rs[:, tc:tc + 1], in_=ex[:, tc, :], axis=AX.X), reads=[Bex], writes=[Brs])
            S.dve(lambda e, tc=tc: e.reciprocal(out=rs[:, tc:tc + 1], in_=rs[:, tc:tc + 1]), reads=[Brs], writes=[Brs])
            S.dve(lambda e, tc=tc: e.tensor_scalar(out=ex[:, tc, :], in0=ex[:, tc, :], scalar1=rs[:, tc:tc + 1], scalar2=None, op0=ALU.mult),
                  reads=[Bex, Brs], writes=[Bex])
        for tc in range(4):
            p_, Bp = C["ps_misc"][tc % 2]
            S.pe(lambda e, tc=tc, p_=p_: e.transpose(p_[0:32, 0:128], ex[:, tc, :], ident[:]), reads=[Bex, Bconst], writes=[Bp])
            S.act(lambda e, tc=tc, p_=p_: e.copy(out=gwT[:, tc * 128:(tc + 1) * 128], in_=p_[0:32, 0:128]), reads=[Bp], writes=[BgwT])
        for c in range(16):
            S.act(lambda e, c=c: e.mul(z[:, c, :], z[:, c, :], ALPHA), reads=[Bz[c]], writes=[Bz[c]])
        for e_ in range(NE):
            gw_t, Bgw = gwb[e_ % 2]
            p_, Bp = C["ps_misc"][e_ % 2]
            S.pe(lambda e, e_=e_, p_=p_: e.matmul(p_[:], sel[:, e_, :], gwT[:], start=True, stop=True),
                 reads=[BgwT, Bconst], writes=[Bp])
            S.act(lambda e, p_=p_, gw_t=gw_t: e.copy(out=gw_t[:], in_=p_[:]), reads=[Bp], writes=[Bgw])
            k = 0
            for j in range(4):
                wg, Bwg = ws.get(req[(tt, "g", e_, j)])
                wu, Bwu = ws.get(req[(tt, "u", e_, j)], hold=2)
                for fl in range(4):
                    f = j * 4 + fl
                    pg, Bpg = nextps()
                    pu, Bpu = nextps()

                    def mmg(e, w=wg, fl=fl, p_=pg):
                        ins = None
                        for kc in range(16):
                            ins = e.matmul(p_[:], w[:, kc, fl * 128:(fl + 1) * 128], hb[:, kc, :], start=(kc == 0), stop=(kc == 15))
                        return ins

                    def mmu(e, w=wu, fl=fl, p_=pu):
                        ins = None
                        for kc in range(16):
                            ins = e.matmul(p_[:], w[:, kc, fl * 128:(fl + 1) * 128], hb[:, kc, :], start=(kc == 0), stop=(kc == 15))
                        return ins
                    S.pe(mmg, reads=[Bwg] + Bhb, writes=[Bpg])
                    S.pe(mmu, reads=[Bwu] + Bhb, writes=[Bpu])
                    g_t, Bg = gs[k % 2]
                    s_t, Bs = sg[k % 2]
                    u_t, Bu = us[k % 2]
                    k += 1
                    S.dve(lambda e, f=f, e_=e_, pg=pg, g_t=g_t: e.tensor_scalar(out=g_t[:], in0=pg[:], scalar1=bgu[:, e_, f:f + 1], scalar2=7.0,
                                                                               op0=ALU.add, op1=ALU.min),
                          reads=[Bpg, Bconst], writes=[Bg])
                    S.act(lambda e, g_t=g_t, s_t=s_t: e.activation(out=s_t[:], in_=g_t[:], func=AF.Sigmoid, scale=1.702),
                          reads=[Bg], writes=[Bs])
                    S.dve(lambda e, f=f, e_=e_, pu=pu, u_t=u_t: e.tensor_scalar(out=u_t[:], in0=pu[:], scalar1=bgu[:, e_, 16 + f:17 + f], scalar2=7.0,
                                                                               op0=ALU.add, op1=ALU.min),
                          reads=[Bpu, Bconst], writes=[Bu])
                    S.dve(lambda e, u_t=u_t: e.tensor_scalar(out=u_t[:], in0=u_t[:], scalar1=-7.0, scalar2=1.0, op0=ALU.max, op1=ALU.add),
                          reads=[Bu], writes=[Bu])
                    S.dve(lambda e, g_t=g_t, s_t=s_t: e.tensor_tensor(out=g_t[:], in0=g_t[:], in1=s_t[:], op=ALU.mult),
                          reads=[Bg, Bs], writes=[Bg])
                    S.dve(lambda e, f=f, g_t=g_t, u_t=u_t: e.tensor_tensor(out=ab[:, f, :], in0=g_t[:], in1=u_t[:], op=ALU.mult),
                          reads=[Bg, Bu], writes=[Bab[f]])
            for j in range(4):
                wdt, Bwd = ws.get(req[(tt, "d", e_, j)])
                for dl in range(4):
                    dc = j * 4 + dl
                    py, Bpy = nextps()

                    def mmd(e, w=wdt, dl=dl, p_=py):
                        ins = None
                        for fc in range(16):
                            ins = e.matmul(p_[:], w[:, fc, dl * 128:(dl + 1) * 128], ab[:, fc, :], start=(fc == 0), stop=(fc == 15))
                        return ins
                    S.pe(mmd, reads=[Bwd] + Bab, writes=[Bpy])
                    t_t, Bt = tmp[dc % 2]
                    S.dve(lambda e, dc=dc, e_=e_, py=py, t_t=t_t, gw_t=gw_t: e.scalar_tensor_tensor(
                        out=t_t[:], in0=py[:], scalar=bd[:, e_, dc:dc + 1], in1=gw_t[:], op0=ALU.add, op1=ALU.mult),
                        reads=[Bpy, Bgw, Bconst], writes=[Bt])
                    S.dve(lambda e, dc=dc, t_t=t_t: e.tensor_tensor(out=z[:, dc, :], in0=z[:, dc, :], in1=t_t[:], op=ALU.add),
                          reads=[Bz[dc], Bt], writes=[Bz[dc]])
        layer_norm_fm(S, nc, C, z, Bz, lnp[:, 2, :], lnp[:, 3, :])
        osrc = IO["hT"].rearrange("(c p) n -> p c n", p=128)
        for h in range(2):
            outs.append(S.dma(lambda e, h=h, tsl=tsl: e.dma_start(out=osrc[:, 8 * h:8 * h + 8, tsl], in_=z[:, 8 * h:8 * h + 8, :]),
                              reads=Bz[8 * h:8 * h + 8]))
    return outs


import math

D = 2048
TT = 512
SCALE = 128 ** -0.5
RMS_EPS = 1e-5
TWO_PI = 2.0 * math.pi


def rope_tables(S, nc, T, pos_ap, tsl):
    posb, Bposb = T["posb"]
    cosT, Bcos = T["cosT"]
    sinT, Bsin = T["sinT"]
    ang, Bang = T["ang"]
    ki, Bki = T["ki"]
    kf, Bkf = T["kf"]
    S.dma(lambda e: e.dma_start(out=posb[:], in_=pos_ap[0:1, tsl].partition_broadcast(128)), writes=[Bposb], q="pool")
    for which in range(2):
        phase = 0.25 if which == 0 else 0.0
        S.dve(lambda e, phase=phase: e.tensor_scalar(out=ang[:], in0=posb[:], scalar1=T["invf"][:, 0:1], scalar2=phase, op0=ALU.mult, op1=ALU.add),
              reads=[Bposb, T["Bconst"]], writes=[Bang])
        S.dve(lambda e: e.tensor_copy(out=ki[:], in_=ang[:]), reads=[Bang], writes=[Bki])
        S.dve(lambda e: e.tensor_copy(out=kf[:], in_=ki[:]), reads=[Bki], writes=[Bkf])
        S.dve(lambda e: e.tensor_tensor(out=ang[:], in0=ang[:], in1=kf[:], op=ALU.subtract), reads=[Bang, Bkf], writes=[Bang])
        S.dve(lambda e: e.tensor_scalar(out=kf[:], in0=ang[:], scalar1=0.5, scalar2=None, op0=ALU.is_gt), reads=[Bang], writes=[Bkf])
        S.dve(lambda e: e.tensor_tensor(out=ang[:], in0=ang[:], in1=kf[:], op=ALU.subtract), reads=[Bang, Bkf], writes=[Bang])
        if which == 0:
            S.act(lambda e: e.activation(out=cosT[:], in_=ang[:], func=AF.Sin, scale=TWO_PI), reads=[Bang], writes=[Bcos])
        else:
            S.act(lambda e: e.activation(out=sinT[:], in_=ang[:], func=AF.Sin, scale=T["invf"][:, 1:2]), reads=[Bang, T["Bconst"]], writes=[Bsin])


def emit_diff(S, nc, IO, SEQ, NH, lambda_init):
    nt = SEQ // TT
    nkc = SEQ // 128
    A = lambda n, sh, dt: nc.alloc_sbuf_tensor("sd_" + n, sh, dt)
    QT = [A(f"QT{m}", [128, SEQ], BF16) for m in range(2)]
    KT = [A(f"KT{m}", [128, SEQ], BF16) for m in range(2)]
    Vt = A("Vt", [128, nkc, 256], BF16)
    Wqk = A("Wqk", [128, 16, 1024], BF16)
    Wv = A("Wv", [128, 16, 256], BF16)
    xt = A("xt", [128, 16, TT], BF16)
    invf = A("invf", [128, 2], F32)
    subg = A("subg", [128, 2], F32)
    cm = A("cm", [128, 896], BF16)
    ones_b = A("ones_b", [128, 128], BF16)
    ones_f = A("ones_f", [128, 128], F32)
    lamp = A("lamp", [1, 512], F32)
    lsc = A("lsc", [1, 8], F32)
    nlam = A("nlam", [128, 1], F32)
    Bconst = S.buf("const")
    T = {"posb": (A("posb", [128, TT], F32), S.buf()), "cosT": (A("cosT", [128, TT], F32), S.buf()),
         "sinT": (A("sinT", [128, TT], F32), S.buf()), "ang": (A("ang", [128, TT], F32), S.buf()),
         "ki": (A("ki", [128, TT], I32), S.buf()), "kf": (A("kf", [128, TT], F32), S.buf()),
         "invf": invf, "Bconst": Bconst}
    t1 = (A("t1", [128, TT], F32), S.buf())
    t2 = (A("t2", [128, TT], F32), S.buf())
    pT = [(A(f"pT{i}", [128, TT], BF16), S.buf()) for i in range(3)]
    rr = [(A(f"rr{m}", [128, TT], F32), S.buf()) for m in range(2)]
    om = [[(A(f"om{m}{c}", [128, TT], F32), S.buf()) for c in range(2)] for m in range(2)]
    at = [(A(f"at{c}", [128, TT], F32), S.buf()) for c in range(2)]
    sq = [(A(f"sq{c}", [128, TT], F32), S.buf()) for c in range(2)]
    rms = (A("rms", [128, TT], F32), S.buf())
    ps = [nc.alloc_psum_tensor(f"pd{i}", [128, 512], F32) for i in range(8)]
    Bps = S.bufs(8, "pd")
    BQT = [S.bufs(nt, f"QT{m}_") for m in range(2)]
    BKT = [S.bufs(nt, f"KT{m}_") for m in range(2)]
    BVt = S.bufs(nt, "Vt")
    BW, Bxt = S.buf("W"), S.buf("xt")
    Blam = S.buf("lam")

    S.dma(lambda e: e.dma_start(out=invf[:], in_=IO["invf"]), writes=[Bconst])
    S.dma(lambda e: e.dma_start(out=subg[:], in_=IO["subg"]), writes=[Bconst])
    S.dma(lambda e: e.dma_start(out=cm[:], in_=IO["cm"]), writes=[Bconst], q="pool")
    S.dma(lambda e: e.dma_start(out=lamp[:], in_=IO["lamp"]), writes=[Blam])
    S.dve(lambda e: e.memset(ones_f[:], 1.0), writes=[Bconst])
    S.dve(lambda e: e.memset(ones_b[:], 1.0), writes=[Bconst])
    S.dve(lambda e: e.tensor_scalar(out=subg[:], in0=subg[:], scalar1=1.0 - lambda_init, scalar2=None, op0=ALU.mult),
          reads=[Bconst], writes=[Bconst])
    S.dve(lambda e: e.tensor_tensor(out=lamp[:, 0:128], in0=lamp[:, 0:128], in1=lamp[:, 128:256], op=ALU.mult), reads=[Blam], writes=[Blam])
    S.dve(lambda e: e.tensor_tensor(out=lamp[:, 256:384], in0=lamp[:, 256:384], in1=lamp[:, 384:512], op=ALU.mult), reads=[Blam], writes=[Blam])
    S.dve(lambda e: e.reduce_sum(out=lsc[:, 0:1], in_=lamp[:, 0:128], axis=AX.X), reads=[Blam], writes=[Blam])
    S.dve(lambda e: e.reduce_sum(out=lsc[:, 1:2], in_=lamp[:, 256:384], axis=AX.X), reads=[Blam], writes=[Blam])
    S.act(lambda e: e.activation(out=lsc[:, 2:4], in_=lsc[:, 0:2], func=AF.Exp), reads=[Blam], writes=[Blam])
    S.dve(lambda e: e.tensor_tensor(out=lsc[:, 4:5], in0=lsc[:, 3:4], in1=lsc[:, 2:3], op=ALU.subtract), reads=[Blam], writes=[Blam])
    S.dve(lambda e: e.tensor_scalar(out=lsc[:, 4:5], in0=lsc[:, 4:5], scalar1=-lambda_init, scalar2=None, op0=ALU.add), reads=[Blam], writes=[Blam])
    S.pe(lambda e: e.matmul(ps[7][:, 0:1], ones_f[0:1, :], lsc[0:1, 4:5], start=True, stop=True), reads=[Blam, Bconst], writes=[Bps[7]])
    S.dve(lambda e: e.tensor_copy(out=nlam[:], in_=ps[7][:, 0:1]), reads=[Bps[7]], writes=[Blam])

    xsrc = IO["xT"].rearrange("(c p) n -> p c n", p=128)
    osrc = IO["aT"]
    outs = []
    pc = [0]

    def nps(k=6):
        i = pc[0] % k
        pc[0] += 1
        return ps[i], Bps[i]
    sc = [0]

    def nps_s():
        i = 6 + sc[0] % 2
        sc[0] += 1
        return ps[i], Bps[i]
    oc = [0]

    for h in range(NH):
        wsrc = IO["wqk"][h].rearrange("(c p) n -> p c n", p=128)
        vsrc = IO["wv"][h].rearrange("(c p) n -> p c n", p=128)
        for q4 in range(4):
            S.dma(lambda e, q4=q4, wsrc=wsrc: e.dma_start(out=Wqk[:, 4 * q4:4 * q4 + 4, :], in_=wsrc[:, 4 * q4:4 * q4 + 4, :]), writes=[BW], q="pool")
        S.dma(lambda e, vsrc=vsrc: e.dma_start(out=Wv[:], in_=vsrc), writes=[BW], q="pool")
        for tt in range(nt):
            tsl = slice(tt * TT, (tt + 1) * TT)
            for q2 in range(2):
                S.dma(lambda e, q2=q2, tsl=tsl: e.dma_start(out=xt[:, 8 * q2:8 * q2 + 8, :], in_=xsrc[:, 8 * q2:8 * q2 + 8, tsl]), writes=[Bxt], q="pool")
            rope_tables(S, nc, T, IO["pos"], tsl)
            cosT, Bcos = T["cosT"]
            sinT, Bsin = T["sinT"]
            for i in range(4):
                dst, Bdst = ((QT, BQT) if i < 2 else (KT, BKT))
                dst, Bdst = dst[i % 2], Bdst[i % 2][tt]
                pa, Bpa = nps()
                pb, Bpb = nps()

                def mm(e, p_, col):
                    ins = None
                    for kc in range(16):
                        ins = e.matmul(p_[:], Wqk[:, kc, col * 128:(col + 1) * 128], xt[:, kc, :], start=(kc == 0), stop=(kc == 15))
                    return ins
                S.pe(lambda e, pa=pa, i=i: mm(e, pa, 2 * i), reads=[BW, Bxt], writes=[Bpa])
                S.pe(lambda e, pb=pb, i=i: mm(e, pb, 2 * i + 1), reads=[BW, Bxt], writes=[Bpb])
                S.dve(lambda e, pa=pa: e.tensor_tensor(out=t1[0][:], in0=pa[:], in1=cosT[:], op=ALU.mult), reads=[Bpa, Bcos], writes=[t1[1]])
                S.dve(lambda e, pb=pb: e.tensor_tensor(out=t2[0][:], in0=pb[:], in1=sinT[:], op=ALU.mult), reads=[Bpb, Bsin], writes=[t2[1]])
                S.dve(lambda e, dst=dst, tsl=tsl: e.tensor_tensor(out=dst[:, tsl], in0=t1[0][:], in1=t2[0][:], op=ALU.add),
                      reads=[t1[1], t2[1]], writes=[Bdst])
            for tc in range(4):
                pv, Bpv = nps()

                def mmv(e, pv=pv, tc=tc):
                    ins = None
                    for kc in range(16):
                        ins = e.matmul(pv[:, 0:256], xt[:, kc, tc * 128:(tc + 1) * 128], Wv[:, kc, :], start=(kc == 0), stop=(kc == 15))
                    return ins
                S.pe(mmv, reads=[BW, Bxt], writes=[Bpv])
                S.act(lambda e, pv=pv, tc=tc, tt=tt: e.copy(out=Vt[:, tt * 4 + tc, :], in_=pv[:, 0:256]), reads=[Bpv], writes=[BVt[tt]])
        for qb in range(nt):
            qsl = slice(qb * TT, (qb + 1) * TT)
            nk = 4 * qb + 4
            for m in range(2):
                ob = 3 * (oc[0] % 2)
                oc[0] += 1
                po = [(ps[ob + i], Bps[ob + i]) for i in range(3)]
                for kc in range(nk):
                    psS, BpsS = nps_s()
                    kt = kc // 4
                    S.pe(lambda e, psS=psS, m=m, kc=kc, qsl=qsl: e.matmul(psS[:], KT[m][:, kc * 128:(kc + 1) * 128], QT[m][:, qsl], start=True, stop=True),
                         reads=[BKT[m][kt], BQT[m][qb]], writes=[BpsS])
                    p_t, Bp_t = pT[kc % 3]
                    S.act(lambda e, psS=psS, p_t=p_t: e.activation(out=p_t[:], in_=psS[:], func=AF.Exp, scale=SCALE), reads=[BpsS], writes=[Bp_t])
                    if kc >= 4 * qb:
                        o = (kc - 4 * qb) * 128
                        S.dve(lambda e, p_t=p_t, o=o: e.tensor_tensor(out=p_t[:], in0=p_t[:], in1=cm[:, 384 - o:384 - o + 512], op=ALU.mult),
                              reads=[Bp_t, Bconst], writes=[Bp_t])

                    def pv(e, p_t=p_t, kc=kc, po=po, nk=nk):
                        e.matmul(po[0][0][:], Vt[:, kc, 0:128], p_t[:], start=(kc == 0), stop=(kc == nk - 1))
                        e.matmul(po[1][0][:], Vt[:, kc, 128:256], p_t[:], start=(kc == 0), stop=(kc == nk - 1))
                        return e.matmul(po[2][0][:], ones_b[:], p_t[:], start=(kc == 0), stop=(kc == nk - 1))
                    S.pe(pv, reads=[Bp_t, BVt[kt], Bconst], writes=[po[0][1], po[1][1], po[2][1]])
                r_t, Br = rr[m]
                S.dve(lambda e, r_t=r_t, po=po: e.reciprocal(out=r_t[:], in_=po[2][0][:]), reads=[po[2][1]], writes=[Br])
                for c in range(2):
                    o_t, Bo = om[m][c]
                    S.dve(lambda e, o_t=o_t, po=po, c=c, r_t=r_t: e.tensor_tensor(out=o_t[:], in0=po[c][0][:], in1=r_t[:], op=ALU.mult),
                          reads=[po[c][1], Br], writes=[Bo])
            for c in range(2):
                S.dve(lambda e, c=c: e.scalar_tensor_tensor(out=at[c][0][:], in0=om[1][c][0][:], scalar=nlam[:, 0:1], in1=om[0][c][0][:],
                                                            op0=ALU.mult, op1=ALU.add),
                      reads=[om[1][c][1], om[0][c][1], Blam], writes=[at[c][1]])
                S.dve(lambda e, c=c: e.tensor_tensor(out=sq[c][0][:], in0=at[c][0][:], in1=at[c][0][:], op=ALU.mult), reads=[at[c][1]], writes=[sq[c][1]])
            pss, Bpss = nps_s()

            def mms(e, pss=pss):
                e.matmul(pss[:], ones_f[:], sq[0][0][:], start=True, stop=False)
                return e.matmul(pss[:], ones_f[:], sq[1][0][:], start=False, stop=True)
            S.pe(mms, reads=[sq[0][1], sq[1][1], Bconst], writes=[Bpss])
            S.dve(lambda e, pss=pss: e.tensor_scalar(out=rms[0][:], in0=pss[:], scalar1=1.0 / 256, scalar2=RMS_EPS, op0=ALU.mult, op1=ALU.add),
                  reads=[Bpss], writes=[rms[1]])
            S.act(lambda e: e.sqrt(out=rms[0][:], in_=rms[0][:]), reads=[rms[1]], writes=[rms[1]])
            S.dve(lambda e: e.reciprocal(out=rms[0][:], in_=rms[0][:]), reads=[rms[1]], writes=[rms[1]])
            for c in range(2):
                S.dve(lambda e, c=c: e.scalar_tensor_tensor(out=at[c][0][:], in0=at[c][0][:], scalar=subg[:, c:c + 1], in1=rms[0][:],
                                                            op0=ALU.mult, op1=ALU.mult),
                      reads=[at[c][1], rms[1], Bconst], writes=[at[c][1]])
                r0 = h * 256 + c * 128
                outs.append(S.dma(lambda e, c=c, r0=r0, qsl=qsl: e.dma_start(out=osrc[r0:r0 + 128, qsl], in_=at[c][0][:]), reads=[at[c][1]]))
    return outs


import math

D = 2048
TT = 512
SCALE = 128 ** -0.5
BIGS = (1.0e4, 2.0e4, 4.0e4)


def emit_nsa(S, nc, IO, SEQ):
    nt = SEQ // TT
    nkc = SEQ // 128
    ncmp = SEQ // 16 - 1
    nch_all = (ncmp + 127) // 128
    AR = Arena(nc, 204 * 1024, "nsa_arena")
    A = lambda n, sh, dt: AR.alloc(sh, dt)
    KsT = A("KsT", [128, SEQ], BF16)
    KwT = A("KwT", [128, SEQ], BF16)
    Vs = A("Vs", [128, nkc, 128], BF16)
    Vw = A("Vw", [128, nkc, 128], BF16)
    xt = A("xt", [128, 16, TT], BF16)
    invf = A("invf", [128, 2], F32)
    ones_b = A("ones_b", [128, 128], BF16)
    ones_l = A("ones_l", [128, 128], BF16)
    kcT = A("kcT", [128, 512], BF16)
    vc = A("vc", [128, 4, 128], BF16)
    Bconst = S.buf("const")
    T = {"posb": (A("posb", [128, TT], F32), S.buf()), "cosT": (A("cosT", [128, TT], F32), S.buf()),
         "sinT": (A("sinT", [128, TT], F32), S.buf()), "ang": (A("ang", [128, TT], F32), S.buf()),
         "ki": (A("ki", [128, TT], I32), S.buf()), "kf": (A("kf", [128, TT], F32), S.buf()),
         "invf": invf, "Bconst": Bconst}
    t1 = (A("t1", [128, TT], F32), S.buf())
    t2 = (A("t2", [128, TT], F32), S.buf())
    ps = [nc.alloc_psum_tensor(f"pn{i}", [128, 512], F32) for i in range(8)]
    Bps = S.bufs(8, "pn")
    BK = {n: S.bufs(nt, n) for n in ("Kc", "Vc", "Ks", "Kw", "Vs", "Vw")}
    BW, Bxt, Bcmp = S.buf("W"), S.buf("xt"), S.buf("cmp")
    mark0 = AR.mark()
    KcT = A("KcT", [128, SEQ], BF16)
    VcT = A("VcT", [128, SEQ], BF16)
    Wk = A("Wk", [128, 16, 768], BF16)
    Wv = A("Wv", [128, 16, 384], BF16)
    W1 = A("W1", [128, 2, 32, 128], BF16)
    W2 = A("W2", [128, 2, 128], BF16)
    cposT = A("cposT", [128, 2, 32], BF16)
    cb = A("cb", [128, 2], F32)
    sT = A("sT", [128, 512], BF16)

    def ld(dst, src, q="sp", w=Bconst):
        S.dma(lambda e: e.dma_start(out=dst, in_=src), writes=[w], q=q)

    ld(invf[:], IO["invf"])
    ld(ones_l[:], IO["ones_l"], "pool")
    S.dve(lambda e: e.memset(ones_b[:], 1.0), writes=[Bconst])
    for q2 in range(2):
        ld(Wk[:, 8 * q2:8 * q2 + 8, :], IO["wk"].rearrange("(c p) n -> p c n", p=128)[:, 8 * q2:8 * q2 + 8, :], "pool", BW)
    ld(Wv[:], IO["wv"].rearrange("(c p) n -> p c n", p=128), "pool", BW)
    for br in range(2):
        ld(W1[:, br, :, :], IO["w1"][br].rearrange("(l p) n -> p l n", p=128), "pool", BW)
        ld(W2[:, br, :], IO["w2"][br], "pool", BW)
        ld(cposT[:, br, :], IO["cposT"][br], "pool", BW)

    xsrc = IO["hT"].rearrange("(c p) n -> p c n", p=128)
    pc = [0]

    def nps(k=6):
        i = pc[0] % k
        pc[0] += 1
        return ps[i], Bps[i]
    sc = [0]

    def nps_s():
        i = 6 + sc[0] % 2
        sc[0] += 1
        return ps[i], Bps[i]

    def load_x(tt):
        tsl = slice(tt * TT, (tt + 1) * TT)
        for q2 in range(2):
            S.dma(lambda e, q2=q2: e.dma_start(out=xt[:, 8 * q2:8 * q2 + 8, :], in_=xsrc[:, 8 * q2:8 * q2 + 8, tsl]), writes=[Bxt], q="pool")
        rope_tables(S, nc, T, IO["pos"], tsl)

    def proj_rope(W, col, dst_ap, Bdst, BWx=None):
        BWx = BWx or BW
        pa, Bpa = nps()
        pb, Bpb = nps()

        def mm(e, p_, c_):
            ins = None
            for kc in range(16):
                ins = e.matmul(p_[:], W[:, kc, c_ * 128:(c_ + 1) * 128], xt[:, kc, :], start=(kc == 0), stop=(kc == 15))
            return ins
        S.pe(lambda e: mm(e, pa, col), reads=[BWx, Bxt], writes=[Bpa])
        S.pe(lambda e: mm(e, pb, col + 1), reads=[BWx, Bxt], writes=[Bpb])
        S.dve(lambda e: e.tensor_tensor(out=t1[0][:], in0=pa[:], in1=T["cosT"][0][:], op=ALU.mult), reads=[Bpa, T["cosT"][1]], writes=[t1[1]])
        S.dve(lambda e: e.tensor_tensor(out=t2[0][:], in0=pb[:], in1=T["sinT"][0][:], op=ALU.mult), reads=[Bpb, T["sinT"][1]], writes=[t2[1]])
        S.dve(lambda e: e.tensor_tensor(out=dst_ap, in0=t1[0][:], in1=t2[0][:], op=ALU.add), reads=[t1[1], t2[1]], writes=[Bdst])

    for tt in range(nt):
        tsl = slice(tt * TT, (tt + 1) * TT)
        load_x(tt)
        proj_rope(Wk, 0, KcT[:, tsl], BK["Kc"][tt])
        proj_rope(Wk, 2, KsT[:, tsl], BK["Ks"][tt])
        proj_rope(Wk, 4, KwT[:, tsl], BK["Kw"][tt])
        pv, Bpv = nps()

        def mmvc(e, pv=pv):
            ins = None
            for kc in range(16):
                ins = e.matmul(pv[:], Wv[:, kc, 0:128], xt[:, kc, :], start=(kc == 0), stop=(kc == 15))
            return ins
        S.pe(mmvc, reads=[BW, Bxt], writes=[Bpv])
        S.act(lambda e, pv=pv, tsl=tsl: e.copy(out=VcT[:, tsl], in_=pv[:]), reads=[Bpv], writes=[BK["Vc"][tt]])
        for tc_ in range(4):
            pv, Bpv = nps()

            def mmv(e, pv=pv, tc_=tc_):
                ins = None
                for kc in range(16):
                    ins = e.matmul(pv[:, 0:256], xt[:, kc, tc_ * 128:(tc_ + 1) * 128], Wv[:, kc, 128:384], start=(kc == 0), stop=(kc == 15))
                return ins
            S.pe(mmv, reads=[BW, Bxt], writes=[Bpv])
            S.act(lambda e, pv=pv, tc_=tc_, tt=tt: e.copy(out=Vs[:, tt * 4 + tc_, :], in_=pv[:, 0:128]), reads=[Bpv], writes=[BK["Vs"][tt]])
            S.act(lambda e, pv=pv, tc_=tc_, tt=tt: e.copy(out=Vw[:, tt * 4 + tc_, :], in_=pv[:, 128:256]), reads=[Bpv], writes=[BK["Vw"][tt]])

    S.dve(lambda e: e.memset(kcT[:], 0.0), writes=[Bcmp])
    S.dve(lambda e: e.memset(vc[:], 0.0), writes=[Bcmp])
    for br, src, Bsrc in ((0, KcT, BK["Kc"]), (1, VcT, BK["Vc"])):
        pb_, Bpb_ = nps()

        def mmb(e, pb_=pb_, br=br):
            ins = None
            for l in range(32):
                ins = e.matmul(pb_[:, 0:1], W1[:, br, l, :], cposT[:, br, l:l + 1], start=(l == 0), stop=(l == 31))
            return ins
        S.pe(mmb, reads=[BW], writes=[Bpb_])
        S.dve(lambda e, pb_=pb_, br=br: e.tensor_copy(out=cb[:, br:br + 1], in_=pb_[:, 0:1]), reads=[Bpb_], writes=[Bcmp])
        ph, Bph = nps()

        def mmh(e, ph=ph, br=br, src=src):
            ins = None
            for l in range(32):
                ins = e.matmul(ph[:, 0:ncmp], W1[:, br, l, :], src[:, l:l + 16 * (ncmp - 1) + 1:16], start=(l == 0), stop=(l == 31))
            return ins
        S.pe(mmh, reads=[BW] + Bsrc, writes=[Bph])
        S.act(lambda e, ph=ph, br=br: e.activation(out=sT[:, 0:ncmp], in_=ph[:, 0:ncmp], func=AF.Silu, bias=cb[:, br:br + 1], scale=1.0),
              reads=[Bph, Bcmp], writes=[Bcmp])
        if br == 0:
            p2, Bp2 = nps()
            S.pe(lambda e, p2=p2: e.matmul(p2[:, 0:ncmp], W2[:, 0, :], sT[:, 0:ncmp], start=True, stop=True), reads=[BW, Bcmp], writes=[Bp2])
            S.act(lambda e, p2=p2: e.copy(out=kcT[:, 0:ncmp], in_=p2[:, 0:ncmp]), reads=[Bp2], writes=[Bcmp])
        else:
            for nch in range(nch_all):
                n1 = min(ncmp, (nch + 1) * 128) - nch * 128
                p2, Bp2 = nps()
                S.pe(lambda e, p2=p2, nch=nch, n1=n1: e.matmul(p2[0:n1, 0:128], sT[:, nch * 128:nch * 128 + n1], W2[:, 1, :], start=True, stop=True),
                     reads=[BW, Bcmp], writes=[Bp2])
                S.act(lambda e, p2=p2, nch=nch, n1=n1: e.copy(out=vc[0:n1, nch, :], in_=p2[0:n1, 0:128]), reads=[Bp2], writes=[Bcmp])

    S.barrier()
    AR.reset(mark0)
    Wq = A("Wq", [128, 16, 1024], BF16)
    Wg = A("Wg", [128, 16, 12], BF16)
    ex = A("ex", [128, 64, 128], BF16)
    cm = A("cm", [128, 896], BF16)
    wm = A("wm", [128, 1408], BF16)
    tcm = A("tcm", [128, 2560], BF16)
    rc = A("rc", [128, 4, 129], BF16)
    selg = A("selg", [12, 12, 128], F32)
    fadd = A("fadd", [128, 256], F32)
    fval = A("fval", [128, 256], F32)
    ident = A("ident", [128, 128], F32)
    QTb = [(A(f"QTb{h}", [128, TT], BF16), S.buf()) for h in range(4)]
    gT = (A("gT", [12, TT], F32), S.buf())
    pT = [(A(f"pT{i}", [128, TT], BF16), S.buf()) for i in range(3)]
    ET = [(A(f"ET{i}", [128, TT], BF16), S.buf()) for i in range(4)]
    yh = [(A(f"yh{h}", [128, TT], F32), S.buf()) for h in range(4)]
    rr = (A("rr", [128, TT], F32), S.buf())
    ff = (A("ff", [128, TT], F32), S.buf())
    tm = (A("tm", [128, TT], F32), S.buf())
    imp = [(A(f"imp{q}", [128, 128], F32), S.buf()) for q in range(4)]
    ur = (A("ur", [128, 1], F32), S.buf())
    sc2 = (A("sc2", [128, 128], F32), S.buf())
    m8a = (A("m8a", [128, 8], F32), S.buf())
    m8b = (A("m8b", [128, 8], F32), S.buf())
    smk = (A("smk", [128, 128], F32), S.buf())
    selTb = (A("selTb", [128, TT], BF16), S.buf())
    Bconst2 = S.buf("const2")
    BW2 = S.buf("W2")
    ld(fadd[:], IO["fadd"], "sp", Bconst2)
    ld(fval[:], IO["fval"], "sp", Bconst2)
    ld(ident[:], IO["ident"], "sp", Bconst2)
    ld(selg[:], IO["selg"], "sp", Bconst2)
    ld(cm[:], IO["cm"], "pool", Bconst2)
    ld(wm[:], IO["wm"], "pool", Bconst2)
    ld(tcm[:], IO["tc"], "pool", Bconst2)
    ld(rc[:], IO["rc"], "pool", Bconst2)
    for q4 in range(4):
        ld(ex[:, 16 * q4:16 * q4 + 16, :], IO["ex"][:, 16 * q4:16 * q4 + 16, :], "pool", Bconst2)
    ld(Wg[:], IO["wg"].rearrange("(c p) n -> p c n", p=128), "pool", BW2)
    for q4 in range(4):
        ld(Wq[:, 4 * q4:4 * q4 + 4, :], IO["wq"].rearrange("(c p) n -> p c n", p=128)[:, 4 * q4:4 * q4 + 4, :], "pool", BW2)
    outs = []
    acc = [0]

    def acc_banks():
        ob = 2 * (acc[0] % 2)
        acc[0] += 1
        return (ps[ob], Bps[ob]), (ps[ob + 1], Bps[ob + 1])

    def finalize_branch(h, c, po, prs, first):
        pg, Bpg = ps[4], Bps[4]
        S.pe(lambda e: e.matmul(pg[:], selg[:, h * 3 + c, :], gT[0][:], start=True, stop=True), reads=[gT[1], Bconst2], writes=[Bpg])
        S.dve(lambda e: e.tensor_scalar(out=rr[0][:], in0=prs[0][:], scalar1=1.0e-30, scalar2=None, op0=ALU.max), reads=[prs[1]], writes=[rr[1]])
        S.dve(lambda e: e.reciprocal(out=rr[0][:], in_=rr[0][:]), reads=[rr[1]], writes=[rr[1]])
        S.dve(lambda e: e.tensor_tensor(out=ff[0][:], in0=pg[:], in1=rr[0][:], op=ALU.mult), reads=[Bpg, rr[1]], writes=[ff[1]])
        if first:
            S.dve(lambda e: e.tensor_tensor(out=yh[h][0][:], in0=po[0][:], in1=ff[0][:], op=ALU.mult), reads=[po[1], ff[1]], writes=[yh[h][1]])
        else:
            S.dve(lambda e: e.tensor_tensor(out=tm[0][:], in0=po[0][:], in1=ff[0][:], op=ALU.mult), reads=[po[1], ff[1]], writes=[tm[1]])
            S.dve(lambda e: e.tensor_tensor(out=yh[h][0][:], in0=yh[h][0][:], in1=tm[0][:], op=ALU.add), reads=[yh[h][1], tm[1]], writes=[yh[h][1]])

    for qb in range(nt):
        qsl = slice(qb * TT, (qb + 1) * TT)
        load_x(qb)
        for h in range(4):
            proj_rope(Wq, 2 * h, QTb[h][0][:], QTb[h][1], BW2)
        pgm, Bpgm = ps[5], Bps[5]

        def mmg(e):
            ins = None
            for kc in range(16):
                ins = e.matmul(pgm[0:12, :], Wg[:, kc, :], xt[:, kc, :], start=(kc == 0), stop=(kc == 15))
            return ins
        S.pe(mmg, reads=[BW2, Bxt], writes=[Bpgm])
        S.act(lambda e: e.activation(out=gT[0][:], in_=pgm[0:12, :], func=AF.Sigmoid), reads=[Bpgm], writes=[gT[1]])

        nchv = min(nch_all, qb // 4 + 1)
        for h in range(4):
            po, prs = acc_banks()
            for nch in range(nchv):
                psS, BpsS = nps_s()
                S.pe(lambda e, psS=psS, nch=nch, h=h: e.matmul(psS[:], kcT[:, nch * 128:(nch + 1) * 128], QTb[h][0][:], start=True, stop=True),
                     reads=[Bcmp, QTb[h][1]], writes=[BpsS])
                e_t, Be = ET[nch]
                S.act(lambda e, psS=psS, e_t=e_t: e.activation(out=e_t[:], in_=psS[:], func=AF.Exp, scale=SCALE), reads=[BpsS], writes=[Be])
                u0 = 512 * qb - 2048 * nch
                if u0 < 2064:
                    S.dve(lambda e, e_t=e_t, u0=u0: e.tensor_tensor(out=e_t[:], in0=e_t[:], in1=tcm[:, u0:u0 + 512], op=ALU.mult),
                          reads=[Be, Bconst2], writes=[Be])

                def pvc(e, e_t=e_t, nch=nch, po=po, prs=prs):
                    e.matmul(po[0][:], vc[:, nch, :], e_t[:], start=(nch == 0), stop=(nch == nchv - 1))
                    return e.matmul(prs[0][:], (ones_l if nch == nch_all - 1 else ones_b)[:], e_t[:], start=(nch == 0), stop=(nch == nchv - 1))
                S.pe(pvc, reads=[Be, Bcmp, Bconst2], writes=[po[1], prs[1]])
            finalize_branch(h, 0, po, prs, True)
            for qc in range(4):
                pu, Bpu = ps[5], Bps[5]

                def mmu(e, qc=qc, pu=pu):
                    ins = None
                    for nch in range(nchv):
                        ins = e.matmul(pu[:, 0:129], ET[nch][0][:, qc * 128:(qc + 1) * 128], rc[:, nch, :], start=(nch == 0), stop=(nch == nchv - 1))
                    return ins
                S.pe(mmu, reads=[ET[n][1] for n in range(nchv)] + [Bconst2], writes=[Bpu])
                S.dve(lambda e, pu=pu: e.tensor_scalar(out=ur[0][:], in0=pu[:, 128:129], scalar1=1.0e-30, scalar2=None, op0=ALU.max), reads=[Bpu], writes=[ur[1]])
                S.dve(lambda e: e.reciprocal(out=ur[0][:], in_=ur[0][:]), reads=[ur[1]], writes=[ur[1]])
                if h == 0:
                    S.dve(lambda e, pu=pu, qc=qc: e.tensor_scalar(out=imp[qc][0][:], in0=pu[:, 0:128], scalar1=ur[0][:, 0:1], scalar2=None, op0=ALU.mult),
                          reads=[Bpu, ur[1]], writes=[imp[qc][1]])
                else:
                    S.dve(lambda e, pu=pu, qc=qc: e.scalar_tensor_tensor(out=imp[qc][0][:], in0=pu[:, 0:128], scalar=ur[0][:, 0:1], in1=imp[qc][0][:],
                                                                        op0=ALU.mult, op1=ALU.add),
                          reads=[Bpu, ur[1], imp[qc][1]], writes=[imp[qc][1]])
        for qc in range(4):
            c0 = 2 * (4 * qb + qc)
            s_t, Bs = imp[qc]
            fs = slice(127 - c0, 127 - c0 + 128)
            S.dve(lambda e, s_t=s_t, fs=fs: e.tensor_tensor(out=s_t[:], in0=s_t[:], in1=fadd[:, fs], op=ALU.add), reads=[Bs, Bconst2], writes=[Bs])
            S.dve(lambda e, s_t=s_t: e.tensor_scalar(out=s_t[:, 0:1], in0=s_t[:, 0:1], scalar1=BIGS[2], scalar2=None, op0=ALU.add), reads=[Bs], writes=[Bs])
            S.dve(lambda e, s_t=s_t, fs=fs: e.scalar_tensor_tensor(out=s_t[:], in0=s_t[:], scalar=1.0, in1=fval[:, fs], op0=ALU.add, op1=ALU.mult),
                  reads=[Bs, Bconst2], writes=[Bs])
            S.dve(lambda e, s_t=s_t: e.tensor_scalar(out=s_t[:], in0=s_t[:], scalar1=-1.0, scalar2=None, op0=ALU.add), reads=[Bs], writes=[Bs])
            S.dve(lambda e, s_t=s_t: e.max(out=m8a[0][:], in_=s_t[:]), reads=[Bs], writes=[m8a[1]])
            S.dve(lambda e, s_t=s_t: e.match_replace(out=sc2[0][:], in_to_replace=m8a[0][:], in_values=s_t[:], imm_value=-1.0e30),
                  reads=[Bs, m8a[1]], writes=[sc2[1]])
            S.dve(lambda e: e.max(out=m8b[0][:], in_=sc2[0][:]), reads=[sc2[1]], writes=[m8b[1]])
            S.dve(lambda e, s_t=s_t: e.tensor_scalar(out=smk[0][:], in0=s_t[:], scalar1=m8b[0][:, 7:8], scalar2=None, op0=ALU.is_ge),
                  reads=[Bs, m8b[1]], writes=[smk[1]])
            S.dve(lambda e, fs=fs: e.tensor_tensor(out=smk[0][:], in0=smk[0][:], in1=fval[:, fs], op=ALU.mult), reads=[smk[1], Bconst2], writes=[smk[1]])
            ptr, Bptr = ps[5], Bps[5]
            S.pe(lambda e, ptr=ptr: e.transpose(ptr[:, 0:128], smk[0][:], ident[:]), reads=[smk[1], Bconst2], writes=[Bptr])
            S.act(lambda e, ptr=ptr, qc=qc: e.copy(out=selTb[0][:, qc * 128:(qc + 1) * 128], in_=ptr[:, 0:128]), reads=[Bptr], writes=[selTb[1]])
        for h in range(4):
            for c, KT_, V_, BKn, BVn in ((1, KsT, Vs, "Ks", "Vs"), (2, KwT, Vw, "Kw", "Vw")):
                po, prs = acc_banks()
                k0 = 0 if c == 1 else max(0, 4 * qb - 4)
                k1 = 4 * qb + 4
                for kc in range(k0, k1):
                    kt = kc // 4
                    psS, BpsS = nps_s()
                    S.pe(lambda e, psS=psS, kc=kc, h=h, KT_=KT_: e.matmul(psS[:], KT_[:, kc * 128:(kc + 1) * 128], QTb[h][0][:], start=True, stop=True),
                         reads=[BK[BKn][kt], QTb[h][1]], writes=[BpsS])
                    p_t, Bp_t = pT[kc % 3]
                    S.act(lambda e, psS=psS, p_t=p_t: e.activation(out=p_t[:], in_=psS[:], func=AF.Exp, scale=SCALE), reads=[BpsS], writes=[Bp_t])
                    if c == 1:
                        pm, Bpm = ps[4 + (kc % 2)], Bps[4 + (kc % 2)]
                        S.pe(lambda e, pm=pm, kc=kc: e.matmul(pm[:], ex[:, kc, :], selTb[0][:], start=True, stop=True), reads=[selTb[1], Bconst2], writes=[Bpm])
                        S.dve(lambda e, p_t=p_t, pm=pm: e.tensor_tensor(out=p_t[:], in0=p_t[:], in1=pm[:], op=ALU.mult), reads=[Bp_t, Bpm], writes=[Bp_t])
                        if kc >= 4 * qb:
                            o = (kc - 4 * qb) * 128
                            S.dve(lambda e, p_t=p_t, o=o: e.tensor_tensor(out=p_t[:], in0=p_t[:], in1=cm[:, 384 - o:384 - o + 512], op=ALU.mult),
                                  reads=[Bp_t, Bconst2], writes=[Bp_t])
                    else:
                        rel = 128 * kc - 512 * qb
                        S.dve(lambda e, p_t=p_t, rel=rel: e.tensor_tensor(out=p_t[:], in0=p_t[:], in1=wm[:, 384 - rel:384 - rel + 512], op=ALU.mult),
                              reads=[Bp_t, Bconst2], writes=[Bp_t])

                    def pv_(e, p_t=p_t, kc=kc, po=po, prs=prs, V_=V_, k0=k0, k1=k1):
                        e.matmul(po[0][:], V_[:, kc, :], p_t[:], start=(kc == k0), stop=(kc == k1 - 1))
                        return e.matmul(prs[0][:], ones_b[:], p_t[:], start=(kc == k0), stop=(kc == k1 - 1))
                    S.pe(pv_, reads=[Bp_t, BK[BVn][kt], Bconst2], writes=[po[1], prs[1]])
                finalize_branch(h, c, po, prs, False)
            outs.append(S.dma(lambda e, h=h, qsl=qsl: e.dma_start(out=IO["yT"][h * 128:(h + 1) * 128, qsl], in_=yh[h][0][:]), reads=[yh[h][1]]))
    return outs


import math
from concourse.bass_utils import run_bass_kernel_spmd

SEQ_FULL = 8192
NCORES = 8
_CACHE = {}


def _pp(v, nch):
    return np.ascontiguousarray(np.asarray(v).reshape(nch, 128).T)


def _build_diff():
    nc = bass.Bass("TRN2", target_bir_lowering=False)
    shp = {"xT": [2048, SEQ_FULL], "wqk": [2, 2048, 1024], "wv": [2, 2048, 256], "invf": [128, 2], "lamp": [1, 512], "subg": [128, 2], "cm": [128, 896]}
    IO = {k: nc.dram_tensor(k, s, F32, kind="ExternalInput").ap() for k, s in shp.items()}
    IO["pos"] = nc.dram_tensor("pos", [1, SEQ_FULL], I32, kind="ExternalInput").ap()
    IO["aT"] = nc.dram_tensor("aT", [512, SEQ_FULL], F32, kind="ExternalOutput").ap()
    S = Sched(nc)
    outs = emit_diff(S, nc, IO, SEQ_FULL, 2, 0.8 - 0.6 * math.exp(0.0))
    S.finalize(final_wait_ops=outs)
    return nc


def _build_post():
    NT = 2048
    nc = bass.Bass("TRN2", target_bir_lowering=False)
    shp = {"aT": [2048, NT], "xT": [2048, NT], "wo": [2048, 2048], "lnp": [128, 4, 16], "rw": [2048, 32], "rbb": [128, 32],
           "wgu": [32, 2048, 4096], "bgu": [128, 32, 32], "wd": [32, 2048, 2048], "bd": [128, 32, 16], "ident": [128, 128], "sel": [32, 32, 128]}
    IO = {k: nc.dram_tensor(k, s, F32, kind="ExternalInput").ap() for k, s in shp.items()}
    IO["hT"] = nc.dram_tensor("hT", [2048, NT], F32, kind="ExternalOutput").ap()
    S = Sched(nc)
    outs = emit_post(S, nc, IO, NT)
    S.finalize(final_wait_ops=outs)
    return nc


def _build_nsa():
    nc = bass.Bass("TRN2", target_bir_lowering=False)
    IO = {k: nc.dram_tensor(k, s, F32, kind="ExternalInput").ap() for k, s in NSA_SHAPES(SEQ_FULL).items()}
    IO["pos"] = nc.dram_tensor("pos", [1, SEQ_FULL], I32, kind="ExternalInput").ap()
    IO["yT"] = nc.dram_tensor("yT", [512, SEQ_FULL], F32, kind="ExternalOutput").ap()
    S = Sched(nc)
    outs = emit_nsa(S, nc, IO, SEQ_FULL)
    S.finalize(final_wait_ops=outs)
    return nc


def _post_inputs(inp, li, wo, aT_b, xT_b):
    lng, lnb = np.asarray(inp["ln_gain"][li]), np.asarray(inp["ln_bias"][li])
    lnp = np.ascontiguousarray(np.stack([_pp(lng[0], 16), _pp(lnb[0], 16), _pp(lng[1], 16), _pp(lnb[1], 16)], axis=1))
    shared = {"wo": np.ascontiguousarray(wo), "lnp": lnp, "rw": np.ascontiguousarray(inp["moe_router_w"][li]),
              "rbb": np.ascontiguousarray(np.broadcast_to(np.asarray(inp["moe_router_b"][li])[None, :], (128, 32))),
              "wgu": np.ascontiguousarray(inp["moe_w_gate_up"][li]),
              "bgu": np.ascontiguousarray(np.asarray(inp["moe_b_gate_up"][li]).reshape(32, 32, 128).transpose(2, 0, 1)),
              "wd": np.ascontiguousarray(inp["moe_w_down"][li]),
              "bd": np.ascontiguousarray(np.asarray(inp["moe_b_down"][li]).reshape(32, 16, 128).transpose(2, 0, 1)),
              "ident": np.eye(128, dtype=np.float32),
              "sel": np.ascontiguousarray(np.broadcast_to(np.eye(32, dtype=np.float32)[:, :, None], (32, 32, 128)))}
    maps = []
    for c in range(NCORES):
        b, q = c // 4, c % 4
        tsl = slice(q * 2048, (q + 1) * 2048)
        m = dict(shared)
        m["aT"] = np.ascontiguousarray(aT_b[b][:, tsl])
        m["xT"] = np.ascontiguousarray(xT_b[b][:, tsl])
        maps.append(m)
    return maps


def kernel(**inp):
    cores = list(range(NCORES))
    x = np.asarray(inp["x"], dtype=np.float32)
    pos = np.asarray(inp["positions"]).astype(np.int32)
    B = x.shape[0]
    xT_b = [np.ascontiguousarray(x[b].T) for b in range(B)]
    if "diff" not in _CACHE:
        _CACHE["diff"] = _build_diff()
    w_in = np.asarray(inp["diff_w_in"][0])

    def head_w(h):
        cols = []
        for i in range(4):
            w = w_in[:, i * 1024 + h * 128: i * 1024 + (h + 1) * 128]
            cols += [w, perm(w)]
        return np.concatenate(cols, axis=1), w_in[:, 4096 + h * 256: 4096 + (h + 1) * 256]
    lamp = np.concatenate([inp["diff_lambda_q1"][0], inp["diff_lambda_k1"][0], inp["diff_lambda_q2"][0], inp["diff_lambda_k2"][0]])[None, :]
    shared = {"invf": rope_consts(), "cm": causal_table(), "lamp": np.ascontiguousarray(lamp, dtype=np.float32),
              "subg": _pp(inp["diff_subln_gain"][0], 2)}
    maps = []
    for c in cores:
        b, hp = c // 4, c % 4
        hw = [head_w(2 * hp), head_w(2 * hp + 1)]
        m = dict(shared)
        m.update({"xT": xT_b[b], "wqk": np.ascontiguousarray(np.stack([hw[0][0], hw[1][0]])),
                  "wv": np.ascontiguousarray(np.stack([hw[0][1], hw[1][1]])), "pos": np.ascontiguousarray(pos[b][None, :])})
        maps.append(m)
    res = run_bass_kernel_spmd(_CACHE["diff"], maps, core_ids=cores)
    aT_b = [np.concatenate([res.results[b * 4 + hp]["aT"] for hp in range(4)], axis=0) for b in range(B)]
    if "post" not in _CACHE:
        _CACHE["post"] = _build_post()
    res = run_bass_kernel_spmd(_CACHE["post"], _post_inputs(inp, 0, inp["diff_w_o"][0], aT_b, xT_b), core_ids=cores)
    hT_b = [np.concatenate([res.results[b * 4 + q]["hT"] for q in range(4)], axis=1) for b in range(B)]
    if "nsa" not in _CACHE:
        _CACHE["nsa"] = _build_nsa()
    consts = nsa_consts(SEQ_FULL)
    maps = []
    for c in cores:
        b, g = c // 4, c % 4
        m = dict(consts)
        m.update(nsa_weights(inp, g))
        m.update({"hT": hT_b[b], "pos": np.ascontiguousarray(pos[b][None, :])})
        maps.append(m)
    res = run_bass_kernel_spmd(_CACHE["nsa"], maps, core_ids=cores)
    yT_b = [np.concatenate([res.results[b * 4 + g]["yT"] for g in range(4)], axis=0) for b in range(B)]
    res = run_bass_kernel_spmd(_CACHE["post"], _post_inputs(inp, 1, inp["nsa_w_o"][0], yT_b, hT_b), core_ids=cores)
    out = np.stack([np.concatenate([res.results[b * 4 + q]["hT"] for q in range(4)], axis=1).T for b in range(B)])
    return np.ascontiguousarray(out, dtype=np.float32)
```
